# Optimizing a Trainium2 kernel written in Bass

```python
import math
import jax, jax.numpy as jnp
from jax import lax
import numpy as np

D_MODEL = 2048
BATCH = 4
SEQ = 2048
DEPTH = 4

A_HEADS = 6
A_DK = 128
A_DV = 128
A_CHUNK = 64
B_HEADS = 6
B_DH = 128
B_LATENT = 256
IDX_HEADS = 16
IDX_DIM = 64
TOPK_MAX = 256
Q_BLOCK = 128
C_GROUPS = 4
C_DIM = 128
C_CHUNK = 128
REL_BUCKETS = 32
REL_MAX_DIST = 128
D_FF = 5632
CONV_W = 3
EPS = 1e-6

A_WK = A_HEADS * A_DK
A_WV = A_HEADS * A_DV
B_W = B_HEADS * B_DH
C_W = C_GROUPS * C_DIM
D_MIX = A_WV + B_W + C_W
IN_SIZES = (A_WK, A_WK, A_WV, A_WV, B_W, B_LATENT, IDX_HEADS * IDX_DIM, IDX_DIM, IDX_HEADS, 2 * C_W)
D_IN = A_WK * 2 + A_WV * 2 + B_W + B_LATENT + IDX_HEADS * IDX_DIM + IDX_DIM + IDX_HEADS + 2 * C_W

kernel_name = "hymba_style_hgrn2_dsa_gmlp_trunk"


def rms_norm(x, g):
    x32 = x.astype(jnp.float32)
    y = x32 * lax.rsqrt(jnp.mean(x32 * x32, axis=-1, keepdims=True) + EPS)
    return (y * g.astype(jnp.float32)).astype(x.dtype)


def split_cols(h, sizes):
    outs, off = [], 0
    for s in sizes:
        outs.append(h[..., off:off + s])
        off += s
    return outs


def hgrn2_mixer(q, f_logit, i, g, lb, out_gain):
    f32 = jnp.float32
    bsz, s_len, _ = q.shape
    n = s_len // A_CHUNK
    q = jax.nn.silu(q.astype(f32))
    log_f = jnp.logaddexp(jnp.log(lb), jnp.log1p(-lb) + jax.nn.log_sigmoid(f_logit.astype(f32)))
    k = -jnp.expm1(log_f)

    def chunks(t, d):
        return t.reshape(bsz, n, A_CHUNK, A_HEADS, d).transpose(1, 0, 3, 2, 4)

    qc = chunks(q, A_DK)
    kc = chunks(k, A_DK)
    ic = chunks(i.astype(f32), A_DV)
    gc = jnp.cumsum(chunks(log_f, A_DK), axis=-2)
    causal = jnp.tril(jnp.ones((A_CHUNK, A_CHUNK), dtype=bool))[:, :, None]

    def step(state, xs):
        qb, kb, ib, gb = xs
        diff = jnp.where(causal, gb[..., :, None, :] - gb[..., None, :, :], -jnp.inf)
        attn = jnp.einsum('bhtd,bhsd,bhtsd->bhts', qb, kb, jnp.exp(diff))
        o = (jnp.einsum('bhts,bhsv->bhtv', attn, ib)
             + jnp.einsum('bhtd,bhdv->bhtv', qb * jnp.exp(gb), state))
        g_last = gb[..., -1:, :]
        state = (jnp.exp(g_last[..., 0, :])[..., None] * state
                 + jnp.einsum('bhsd,bhsv->bhdv', kb * jnp.exp(g_last - gb), ib))
        return state, o

    state0 = jnp.zeros((bsz, A_HEADS, A_DK, A_DV), f32)
    _, o = lax.scan(step, state0, (qc, kc, ic, gc))
    o = o.transpose(1, 0, 3, 2, 4).reshape(bsz, s_len, A_HEADS, A_DV)
    gate = jax.nn.silu(g.astype(f32)).reshape(bsz, s_len, A_HEADS, A_DV)
    return (rms_norm(o, out_gain) * gate).reshape(bsz, s_len, A_WV)


def t5_bucket(dist):
    max_exact = REL_BUCKETS // 2
    d = jnp.maximum(dist, 0)
    d_f = jnp.maximum(d, 1).astype(jnp.float32)
    large = max_exact + (jnp.log(d_f / max_exact) / math.log(REL_MAX_DIST / max_exact)
                         * (REL_BUCKETS - max_exact)).astype(jnp.int32)
    large = jnp.minimum(large, REL_BUCKETS - 1)
    return jnp.where(d < max_exact, d, large)


def dsa_mixer(q, c_kv, iq, ik, iw, w_uk, w_uv, kv_gain, rel_bias):
    f32 = jnp.float32
    bsz, s_len, _ = q.shape
    top_k = min(TOPK_MAX, s_len // 4)
    nb = s_len // Q_BLOCK
    c = rms_norm(c_kv, kv_gain).astype(f32)
    q = q.reshape(bsz, s_len, B_HEADS, B_DH).astype(f32)
    q_lat = jnp.einsum('bshd,hcd->bshc', q, w_uk.astype(f32)) * (B_DH ** -0.5)
    iq = iq.reshape(bsz, s_len, IDX_HEADS, IDX_DIM).astype(f32)
    ik = ik.astype(f32)
    iw = iw.astype(f32) * (IDX_HEADS ** -0.5 * IDX_DIM ** -0.5)
    key_pos = jnp.arange(s_len)
    rb = rel_bias.astype(f32)

    def blocks(t):
        return t.reshape(bsz, nb, Q_BLOCK, *t.shape[2:]).swapaxes(0, 1)

    qpos = jnp.arange(s_len).reshape(nb, Q_BLOCK)

    def one_block(xs):
        iq_b, iw_b, ql_b, pos_b = xs
        rel = jax.nn.relu(jnp.einsum('bqhd,bsd->bqhs', iq_b, ik))
        score = jnp.einsum('bqh,bqhs->bqs', iw_b, rel)
        visible = key_pos[None, :] <= pos_b[:, None]
        score = jnp.where(visible[None], score, -jnp.inf)
        _, idx = lax.top_k(score, top_k)
        valid = idx <= pos_b[None, :, None]
        c_sel = jax.vmap(lambda cb, ib: cb[ib])(c, idx)
        logits = jnp.einsum('bqhc,bqkc->bqhk', ql_b, c_sel)
        bias = rb[t5_bucket(pos_b[None, :, None] - idx)]
        logits = logits + bias.transpose(0, 1, 3, 2)
        logits = jnp.where(valid[:, :, None, :], logits, -jnp.inf)
        p = jax.nn.softmax(logits, axis=-1)
        return jnp.einsum('bqhk,bqkc->bqhc', p, c_sel)

    o_lat = lax.map(one_block, (blocks(iq), blocks(iw), blocks(q_lat), qpos))
    o_lat = o_lat.swapaxes(0, 1).reshape(bsz, s_len, B_HEADS, B_LATENT)
    o = jnp.einsum('bshc,hcd->bshd', o_lat, w_uv.astype(f32))
    return o.reshape(bsz, s_len, B_W)


def gmlp_mixer(uv, w_s, b_s):
    f32 = jnp.float32
    bsz, s_len, _ = uv.shape
    n = s_len // C_CHUNK
    uv = jax.nn.gelu(uv.astype(f32))
    u, v = uv[..., :C_W], uv[..., C_W:]
    v = v.reshape(bsz, n, C_CHUNK, C_GROUPS, C_DIM)
    v = v - jnp.mean(v, axis=-1, keepdims=True)
    v = v * lax.rsqrt(jnp.mean(v * v, axis=-1, keepdims=True) + EPS)
    w = w_s.astype(f32) * jnp.tril(jnp.ones((C_CHUNK, C_CHUNK), f32))[None]
    mixed = jnp.einsum('gts,bnsgc->bntgc', w, v) + b_s.astype(f32).T[:, :, None]
    return u * mixed.reshape(bsz, s_len, C_W)


def conv_glu_ffn(h, w_up, conv_w, conv_b, w_down):
    a = h @ w_up
    a = lax.conv_general_dilated(a, conv_w[:, None, :], window_strides=(1,),
                                 padding=[(CONV_W - 1, 0)],
                                 dimension_numbers=('NWC', 'WIO', 'NWC'),
                                 feature_group_count=2 * D_FF) + conv_b
    gate, up = a[..., :D_FF], a[..., D_FF:]
    return (jax.nn.silu(gate) * up) @ w_down


def setup_inputs(seed: int = 0) -> dict:
    key = jax.random.key(seed)
    ks = jax.random.split(key, 20)
    f32 = jnp.float32

    def nrm(k, shape, scale):
        return jax.random.normal(k, shape, f32) * scale

    return {
        "x": nrm(ks[0], (BATCH, SEQ, D_MODEL), 1.0),
        "norm_mix": 1.0 + nrm(ks[1], (DEPTH, D_MODEL), 0.02),
        "w_in": nrm(ks[2], (DEPTH, D_MODEL, D_IN), D_MODEL ** -0.5),
        "lower_bounds": nrm(ks[3], (DEPTH, A_WK), 0.1),
        "a_out_gain": 1.0 + nrm(ks[4], (DEPTH, A_DV), 0.02),
        "kv_gain": 1.0 + nrm(ks[5], (DEPTH, B_LATENT), 0.02),
        "w_uk": nrm(ks[6], (DEPTH, B_HEADS, B_LATENT, B_DH), B_LATENT ** -0.5),
        "w_uv": nrm(ks[7], (DEPTH, B_HEADS, B_LATENT, B_DH), B_LATENT ** -0.5),
        "rel_bias": nrm(ks[8], (REL_BUCKETS, B_HEADS), 0.5),
        "gmlp_w": nrm(ks[9], (DEPTH, C_GROUPS, C_CHUNK, C_CHUNK), C_CHUNK ** -0.5),
        "gmlp_b": 1.0 + nrm(ks[10], (DEPTH, C_GROUPS, C_CHUNK), 0.02),
        "w_out": nrm(ks[11], (DEPTH, D_MIX, D_MODEL), D_MIX ** -0.5),
        "norm_ffn": 1.0 + nrm(ks[12], (DEPTH, D_MODEL), 0.02),
        "w_up": nrm(ks[13], (DEPTH, D_MODEL, 2 * D_FF), D_MODEL ** -0.5),
        "conv_w": nrm(ks[14], (DEPTH, CONV_W, 2 * D_FF), CONV_W ** -0.5),
        "conv_b": nrm(ks[15], (DEPTH, 2 * D_FF), 0.02),
        "w_down": nrm(ks[16], (DEPTH, D_FF, D_MODEL), D_FF ** -0.5),
        "norm_final": 1.0 + nrm(ks[17], (D_MODEL,), 0.02),
    }


def reference(x, norm_mix, w_in, lower_bounds, a_out_gain, kv_gain, w_uk, w_uv, rel_bias,
              gmlp_w, gmlp_b, w_out, norm_ffn, w_up, conv_w, conv_b, w_down, norm_final):
    lb_all = jnp.cumsum(jax.nn.softmax(lower_bounds.astype(jnp.float32), axis=0), axis=0)
    lb_all = lb_all - lb_all[0]
    for l in range(DEPTH):
        h = rms_norm(x, norm_mix[l])
        proj = h @ w_in[l]
        a_q, a_f, a_i, a_g, b_q, b_ckv, idx_q, idx_k, idx_w, c_uv = split_cols(proj, IN_SIZES)
        out_a = hgrn2_mixer(a_q, a_f, a_i, a_g, lb_all[l], a_out_gain[l])
        out_b = dsa_mixer(b_q, b_ckv, idx_q, idx_k, idx_w, w_uk[l], w_uv[l], kv_gain[l], rel_bias)
        out_c = gmlp_mixer(c_uv, gmlp_w[l], gmlp_b[l])
        mix = jnp.concatenate([out_a, out_b, out_c], axis=-1).astype(x.dtype)
        x = x + mix @ w_out[l]
        h = rms_norm(x, norm_ffn[l])
        x = x + conv_glu_ffn(h, w_up[l], conv_w[l], conv_b[l], w_down[l]).astype(x.dtype)
    return rms_norm(x, norm_final)
```

```python
import contextlib
import numpy as np
import concourse.bass as bass
import concourse.mybir as mybir
from concourse.bass_utils import run_bass_kernel_spmd

F32 = mybir.dt.float32
BF16 = mybir.dt.bfloat16
AF = mybir.ActivationFunctionType
ALU = mybir.AluOpType
AX = mybir.AxisListType

D = 2048
S = 2048
DEPTH = 4
DFF = 5632
NFC = 44
EPS = 1e-6
NCORES = 8
ENGS = ["tensor", "vector", "scalar", "gpsimd", "sync"]
EPOCH = 30000
NDMA = 8
NW = 53000

A_Q, A_F, A_I, A_G, B_Q, B_CKV, I_Q, I_K, I_W, C_U, C_V = 0, 768, 1536, 2304, 3072, 3840, 4096, 5120, 5184, 5200, 5712
NB_IN = 50


PSMAP = {"n0ps": [2], "pA": [0, 1, 2, 3], "pB": [4, 5, 6, 7], "pT": [4, 5], "pT4": [4, 5, 6, 7], ("pck", 0): [0, 1, 2, 3], ("pck", 1): [4, 5, 6, 7],
         ("pp", 0): [0], ("pp", 1): [1], "pql": [2], "pl1": [3], "pol": [6], "pov": [7], ("pa", 0): [6], ("pa", 1): [7], ("po", 0): [6], ("po", 1): [7],
         ("pS", 0): [4], ("pS", 1): [5], ("pd", 0): [0], ("pd", 1): [1], "pss": [2], ("pu", 0): [3], ("pu", 1): [4], ("pu", 2): [5], ("pu", 3): [6]}


def _xl(keys):
    out = []
    for k in keys:
        if k in PSMAP:
            out.extend(("pb", b) for b in PSMAP[k])
        else:
            assert not (isinstance(k, str) and k.startswith("p") and k[1:2].isupper() and False)
            out.append(k)
    return out


class Prog:
    def __init__(self, nc):
        self.nc = nc
        self.q = {e: [] for e in ENGS}
        self.res = {}

    def op(self, eng, fn, reads=(), writes=(), dma=False):
        reads = _xl(reads)
        writes = _xl(writes)
        idx = len(self.q[eng])
        deps = set()
        for r in reads:
            st = self.res.get(r)
            if st and st[0] is not None:
                deps.add(st[0])
        for w in writes:
            st = self.res.get(w)
            if st:
                if st[0] is not None:
                    deps.add(st[0])
                for e2j in st[1].values():
                    deps.add(e2j)
        if eng == "tensor":
            deps = {d for d in deps if d[0] != "tensor"}
        rec = dict(fn=fn, deps=deps, signal=False, dma=dma)
        self.q[eng].append(rec)
        for d in deps:
            self.q[d[0]][d[1]]["signal"] = True
        for r in reads:
            st = self.res.setdefault(r, [None, {}])
            st[1][(eng, idx) if dma else eng] = (eng, idx)
        for w in writes:
            self.res[w] = [(eng, idx), {}]
        return (eng, idx)

    def barrier(self):
        lasts = {e: len(self.q[e]) - 1 for e in ENGS if self.q[e]}
        for e in ENGS:
            deps = {(e2, j) for e2, j in lasts.items() if e2 != e and j >= 0}
            for e2 in ("sync", "gpsimd"):
                cnt = 0
                for j in range(len(self.q[e2]) - 1, -1, -1):
                    if self.q[e2][j]["dma"] and self.q[e2][j]["fn"] is not None:
                        deps.add((e2, j))
                        cnt += 1
                        if cnt >= NDMA:
                            break
            real = set()
            for e2, j in deps:
                while j >= 0 and self.q[e2][j]["fn"] is None:
                    j -= 1
                if j >= 0:
                    real.add((e2, j))
                    self.q[e2][j]["signal"] = True
            self.q[e].append(dict(fn=None, deps=real, signal=False, dma=False))
        self.res = {}

    def emit(self, final_waits):
        nc = self.nc
        for e in ENGS:
            c = 0
            d = 0
            for rec in self.q[e]:
                if rec["dma"]:
                    rec["dn"] = d
                    d += 1
                elif rec["signal"]:
                    rec["cnt"] = c
                    c += 1
        with contextlib.ExitStack() as st:
            nsem = {e: (sum(1 for r in self.q[e] if r["signal"] and not r["dma"]) // EPOCH + 1) for e in ENGS}
            sems = {e: [st.enter_context(nc.semaphore(f"s_{e}_{k}")) for k in range(nsem[e])] for e in ENGS}
            dsems = {e: [st.enter_context(nc.semaphore(f"d_{e}_{k}")) for k in range(NDMA)] for e in ("sync", "gpsimd")}
            block = st.enter_context(nc.Block())

            def target(dep):
                e2, j = dep
                r = self.q[e2][j]
                if r["dma"]:
                    n = r["dn"]
                    return (dsems[e2][n % NDMA], 16 * (n // NDMA + 1), ("d", e2, n % NDMA))
                c = r["cnt"]
                return (sems[e2][c // EPOCH], c % EPOCH + 1, ("c", e2, c // EPOCH))

            def run(e, eng):
                waited = {}

                def wait(dep):
                    s, v, key = target(dep)
                    if waited.get(key, 0) >= v:
                        return
                    waited[key] = v
                    eng.wait_ge(s, v)

                for rec in self.q[e]:
                    for dep in sorted(rec["deps"]):
                        wait(dep)
                    if rec["fn"] is None:
                        continue
                    if rec["dma"]:
                        n = rec["dn"]
                        if n >= NDMA:
                            key = ("d", e, n % NDMA)
                            v = 16 * (n // NDMA)
                            if waited.get(key, 0) < v:
                                waited[key] = v
                                eng.wait_ge(dsems[e][n % NDMA], v)
                    ins = rec["fn"](eng)
                    if rec["dma"]:
                        ins.then_inc(dsems[e][rec["dn"] % NDMA], 16)
                    elif rec["signal"]:
                        c = rec["cnt"]
                        ins.then_inc(sems[e][c // EPOCH], 1)
                for dep in final_waits.get(e, ()):
                    s, v, key = target(dep)
                    eng.wait_ge(s, v)

            @block.tensor
            def _(eng):
                run("tensor", eng)

            @block.vector
            def _(eng):
                run("vector", eng)

            @block.scalar
            def _(eng):
                run("scalar", eng)

            @block.gpsimd
            def _(eng):
                run("gpsimd", eng)

            @block.sync
            def _(eng):
                run("sync", eng)


def build(n_layers=DEPTH, layer0=0, final=True, debug=False, stop=None, nb=1):
    nc = bass.Bass("TRN2", target_bir_lowering=False)
    P = Prog(nc)
    L = n_layers

    xT_d = nc.dram_tensor("xT", [nb * D, S], F32, kind="ExternalInput")
    win_d = nc.dram_tensor("win", [L, NB_IN, 128, 2048], F32, kind="ExternalInput")
    lite = stop is not None
    wout_d = nc.dram_tensor("wout", [L, 1 if lite else 16, 128, 2048], F32, kind="ExternalInput")
    wup_d = nc.dram_tensor("wup", [L, 1 if lite else 88, 128, 2048], F32, kind="ExternalInput")
    wdn_d = nc.dram_tensor("wdn", [L, 1 if lite else 64, 128, 11 * 128], F32, kind="ExternalInput")
    NSM = 73
    sm_d = nc.dram_tensor("smalls", [L, 128, NSM], F32, kind="ExternalInput")
    smB_d = nc.dram_tensor("smallsB", [L, 128, 256], F32, kind="ExternalInput")
    smT_d = nc.dram_tensor("smallsT", [L, 128, 352], F32, kind="ExternalInput")
    gb_d = nc.dram_tensor("gbrow", [L, 1, 512], F32, kind="ExternalInput")
    wuk_d = nc.dram_tensor("wukT", [L, 128, 1536], F32, kind="ExternalInput")
    wuv_d = nc.dram_tensor("wuv", [L, 128, 1536], F32, kind="ExternalInput")
    wsT_d = nc.dram_tensor("wsT", [L, 128, 512], F32, kind="ExternalInput")
    bias2_d = nc.dram_tensor("bias2", [128, 1536], F32, kind="ExternalInput")
    c32_d = nc.dram_tensor("c32", [128, 262], F32, kind="ExternalInput")
    c16_d = nc.dram_tensor("c16", [128, 2432], F32, kind="ExternalInput")
    out_d = nc.dram_tensor("outT", [nb * D, S], F32, kind="ExternalOutput")
    xs_d = nc.dram_tensor("xscr", [D, S], F32, kind="Internal")
    if debug:
        dbg_d = nc.dram_tensor("dbg", [128, 16 * 2048], F32, kind="ExternalOutput")

    SB = nc.alloc_sbuf_tensor("SB", [128, NW], F32)
    SBb = SB.bitcast(BF16)
    PS = nc.alloc_psum_tensor("PS", [128, 4096], F32)
    PSb = PS.bitcast(BF16)

    def sb(dt, off, dims, p0=0, npar=128):
        if dt == F32:
            assert off % 4 == 0
            return bass.AP(SB, p0 * NW + off // 4, [[NW, npar]] + [[s, c] for s, c in dims])
        assert off % 2 == 0
        return bass.AP(SBb, p0 * 2 * NW + off // 2, [[2 * NW, npar]] + [[s, c] for s, c in dims])

    def ps(col, dims, dt=F32, p0=0, npar=128):
        if dt == F32:
            return bass.AP(PS, p0 * 4096 + col, [[4096, npar]] + [[s, c] for s, c in dims])
        return bass.AP(PSb, p0 * 8192 + 2 * col, [[8192, npar]] + [[s, c] for s, c in dims])

    def dr(t, off, dims):
        return bass.AP(t, off, [[s, c] for s, c in dims])

    def _mk(name, args, kw):
        return lambda e: getattr(e, name)(*args, **kw)

    def TE(name, *args, r=(), w=(), **kw):
        return P.op("tensor", _mk(name, args, kw), r, w)

    def VE(name, *args, r=(), w=(), **kw):
        return P.op("vector", _mk(name, args, kw), r, w)

    def AE(name, *args, r=(), w=(), **kw):
        return P.op("scalar", _mk(name, args, kw), r, w)

    def GE(name, *args, r=(), w=(), **kw):
        return P.op("gpsimd", _mk(name, args, kw), r, w)

    def dma_sync(out, in_, r=(), w=()):
        return P.op("sync", lambda e: e.dma_start(out=out, in_=in_), r, w, dma=True)

    def dma_pool(out, in_, r=(), w=()):
        return P.op("gpsimd", lambda e: e.dma_start(out=out, in_=in_), r, w, dma=True)

    def mm(out, lhsT, rhs, start, stop, r, w):
        return TE("matmul", out, lhsT, rhs, start=start, stop=stop, r=r, w=w)

    BUF = [0, 65536]
    CB = 131072
    ONES32 = CB
    NEGM = CB + 512
    RBC = CB + 1024
    IDENT = CB + 1056
    BDM = IDENT + 256
    TRIU = BDM + 256
    SCANM = TRIU + 256
    SMALL = SCANM + 4096
    LBT = SMALL + 320
    RB = LBT + 192
    REND = NW * 4
    assert RB % 32 == 0

    def bufT(b, kc, t0, n):
        return sb(BF16, BUF[b] + (kc * 2048 + t0) * 2, [(1, n)])

    dma_sync(sb(F32, ONES32, [(1, 262)]), c32_d.ap(), w=["c32"])
    dma_pool(sb(BF16, IDENT, [(1, 2432)]), c16_d.ap(), w=["c16"])
    ones32 = sb(F32, ONES32, [(1, 128)])
    negm = sb(F32, NEGM, [(1, 128)])
    ident = sb(BF16, IDENT, [(1, 128)])
    bdm = sb(BF16, BDM, [(1, 128)])
    triu = sb(BF16, TRIU, [(1, 128)])
    scanm = sb(BF16, SCANM, [(1, 2048)])

    def rbc(h):
        return sb(F32, RBC + 4 * h, [(1, 1)])

    ws = dict(slots=[], n=0)

    def set_slots(offs):
        ws["slots"] = offs
        ws["n"] = 0

    def next_w(dram_ap, ncols=2048):
        i = ws["n"] % len(ws["slots"])
        ws["n"] += 1
        off = ws["slots"][i]
        key = ("ws", i)
        dma_pool(sb(BF16, off, [(1, ncols)]), dram_ap, w=[key])
        return off, key

    def wblk(t, l, b, ncols=2048):
        return dr(t, (l * t.shape[1] + b) * 128 * ncols, [(ncols, 128), (1, ncols)])

    def rms_rstd(src_fn, nchunks, n, sq_off, pscol, rstd_off, dim, rkeys, tag):
        for kc in range(nchunks):
            AE("activation", sb(F32, sq_off, [(1, n)]), src_fn(kc), AF.Square, r=rkeys(kc), w=[tag + "sq"])
            mm(ps(pscol, [(1, n)]), ones32, sb(F32, sq_off, [(1, n)]), kc == 0, kc == nchunks - 1, [tag + "sq", "c32"], [tag + "ps"])
        AE("activation", sb(F32, rstd_off, [(1, n)]), ps(pscol, [(1, n)]), AF.Sqrt, scale=1.0 / dim, bias=EPS, r=[tag + "ps"], w=[tag + "rstd"])
        VE("reciprocal", sb(F32, rstd_off, [(1, n)]), sb(F32, rstd_off, [(1, n)]), r=[tag + "rstd"], w=[tag + "rstd"])

    def load_smalls(l):
        dma_sync(sb(F32, SMALL, [(1, NSM)]), dr(sm_d, l * 128 * NSM, [(NSM, 128), (1, NSM)]), w=["small"])

    def gcol(which, kc):
        return sb(F32, SMALL + 4 * (16 * which + kc), [(1, 1)])

    for bi in range(nb):
        load_smalls(0)
        XT0 = RB
        SQ0 = RB + 32768
        RS0 = SQ0 + 2048
        for tt in range(4):
            dma_sync(sb(F32, XT0, [(512, 16), (1, 512)]), dr(xT_d, bi * D * S + tt * 512, [(S, 128), (128 * S, 16), (1, 512)]), w=["xt"])
            rms_rstd(lambda kc: sb(F32, XT0 + kc * 2048, [(1, 512)]), 16, 512, SQ0, 1024, RS0, D, lambda kc: ["xt"], "n0")
            for kc in range(16):
                VE("scalar_tensor_tensor", bufT(0, kc, tt * 512, 512), sb(F32, XT0 + kc * 2048, [(1, 512)]), gcol(0, kc),
                                                                 sb(F32, RS0, [(1, 512)]), ALU.mult, ALU.mult,
                   r=["xt", "n0rstd", "small"], w=[("H", kc, tt)])
        P.barrier()

        def finish_early(buf, nchk=16):
            P.barrier()
            dma_pool(dr(dbg_d, 0, [(32768, 128), (1, nchk * 2048)]), sb(BF16, BUF[buf], [(1, nchk * 2048)]), w=["dbg"])
            P.barrier()
            P.emit({})
            return nc

        if stop == "pro":
            return finish_early(0)
        xsrc = xT_d
        for l in range(L):
            lg = layer0 + l
            hb = l % 2
            mbuf = 1 - hb
            if l > 0:
                load_smalls(l)
            LBR = SMALL + 4 * 48
            EX = LBT
            AE("activation", sb(F32, EX, [(1, 24)]), sb(F32, LBR, [(1, 24)]), AF.Exp, r=["small"], w=["lbe"])
            LBV = LBT + 96
            OML = LBT + 120
            DEN = LBT + 144
            VE("tensor_tensor", sb(F32, DEN, [(1, 6)]), sb(F32, EX, [(1, 6)]), sb(F32, EX + 24, [(1, 6)]), ALU.add, r=["lbe"], w=["den"])
            VE("tensor_tensor", sb(F32, DEN, [(1, 6)]), sb(F32, DEN, [(1, 6)]), sb(F32, EX + 48, [(1, 6)]), ALU.add, r=["lbe", "den"], w=["den"])
            VE("tensor_tensor", sb(F32, DEN, [(1, 6)]), sb(F32, DEN, [(1, 6)]), sb(F32, EX + 72, [(1, 6)]), ALU.add, r=["lbe", "den"], w=["den"])
            VE("reciprocal", sb(F32, DEN, [(1, 6)]), sb(F32, DEN, [(1, 6)]), r=["den"], w=["den"])
            VE("memset", sb(F32, LBV, [(1, 6)]), 0.0, w=["lbv"])
            for j in range(1, lg + 1):
                VE("tensor_tensor", sb(F32, LBV, [(1, 6)]), sb(F32, LBV, [(1, 6)]), sb(F32, EX + 24 * j, [(1, 6)]), ALU.add, r=["lbe", "lbv"], w=["lbv"])
            VE("tensor_tensor", sb(F32, LBV, [(1, 6)]), sb(F32, LBV, [(1, 6)]), sb(F32, DEN, [(1, 6)]), ALU.mult, r=["lbv", "den"], w=["lbv"])
            VE("tensor_scalar", sb(F32, OML, [(1, 6)]), sb(F32, LBV, [(1, 6)]), -1.0, 1.0, ALU.mult, ALU.add, r=["lbv"], w=["oml"])

            def Hrhs(kc, t0, n):
                return bufT(hb, kc, t0, n)

            def Hkeys(kc, t0, n):
                return [("H", kc, tt) for tt in range(t0 // 512, (t0 + n - 1) // 512 + 1)]

            def proj_feat(blk, pscol0, key_ps):
                off, key = next_w(wblk(win_d, l, blk))
                for tt in range(4):
                    for kc in range(16):
                        mm(ps(pscol0 + tt * 512, [(1, 512)]), sb(BF16, off + kc * 256, [(1, 128)]), Hrhs(kc, tt * 512, 512), kc == 0, kc == 15,
                           [key, ("H", kc, tt)], [key_ps])

            def proj_tok(blk, pscol0, key_ps, ncol=128):
                off, key = next_w(wblk(win_d, l, blk))
                for tj in range(16):
                    for kc in range(16):
                        mm(ps(pscol0 + tj * ncol, [(1, ncol)]), Hrhs(kc, tj * 128, 128), sb(BF16, off + kc * 256, [(1, ncol)]), kc == 0, kc == 15,
                           [key, ("H", kc, tj // 4)], [key_ps])

            T1 = RB
            T2 = RB + 8192
            T3 = RB + 16384
            BQ = T3 + 4096
            BK = BQ + 4096
            QQ0 = BK + 4096
            B3 = QQ0 + 4096
            QQ1 = B3 + 4096
            KK0 = QQ1 + 4096
            KK1 = KK0 + 4096
            KGT = KK1 + 4096
            ITK = KGT + 4096
            ATT = ITK + 4096
            S32 = ATT + 512
            S16 = S32 + 512
            DEC = S16 + 512
            AEND = DEC + 128
            assert AEND <= REND, AEND
            set_slots([BUF[mbuf] + (6 + i) * 4096 for i in range(8)])
            GE("memset", sb(BF16, QQ1, [(1, 2048)]), 0.0, w=["QQ1"])
            GE("memset", sb(BF16, KK0, [(1, 2048)]), 0.0, w=["KK0"])
            GE("memset", sb(BF16, KK1, [(1, 2048)]), 0.0, w=["KK1"])

            def v3(dt, off, c0, n):
                return sb(dt, off + c0 * (4 if dt == F32 else 2), [(64, 32), (1, n)])

            for h in range(6):
                proj_feat(4 * h + 0, 0, "pA")
                AE("activation", sb(BF16, BQ, [(1, 2048)]), ps(0, [(1, 2048)]), AF.Silu, r=["pA"], w=["BQ"])
                proj_feat(4 * h + 1, 0, "pA")
                AE("activation", sb(F32, T1, [(1, 2048)]), ps(0, [(1, 2048)]), AF.Sigmoid, r=["pA"], w=["T1"])
                AE("activation", sb(F32, T1, [(1, 2048)]), sb(F32, T1, [(1, 2048)]), AF.Ln, scale=sb(F32, OML + 4 * h, [(1, 1)]),
                                              bias=sb(F32, LBV + 4 * h, [(1, 1)]), r=["T1", "lbv", "oml"], w=["T1"])
                VE("tensor_tensor_scan", sb(F32, T2, [(1, 2048)]), scanm, sb(F32, T1, [(1, 2048)]), 0.0, ALU.mult, ALU.add, r=["T1", "c16"], w=["T2"])
                AE("activation", sb(F32, T1, [(1, 2048)]), sb(F32, T1, [(1, 2048)]), AF.Exp, r=["T1"], w=["T1"])
                VE("tensor_scalar", sb(BF16, BK, [(1, 2048)]), sb(F32, T1, [(1, 2048)]), -1.0, 1.0, ALU.mult, ALU.add, r=["T1"], w=["BK"])
                AE("activation", sb(BF16, T3, [(1, 2048)]), sb(F32, T2, [(1, 2048)]), AF.Exp, r=["T2"], w=["T3"])
                VE("tensor_tensor", sb(BF16, QQ0, [(1, 2048)]), sb(BF16, BQ, [(1, 2048)]), sb(BF16, T3, [(1, 2048)]), ALU.mult, r=["BQ", "T3"], w=["QQ0"])
                AE("activation", sb(F32, DEC, [(1, 32)]), sb(F32, T2 + 63 * 4, [(64, 32)]), AF.Exp, r=["T2"], w=["DEC"])
                VE("tensor_tensor", v3(F32, T1, 0, 64), sb(F32, T2 + 63 * 4, [(64, 32), (0, 64)]), v3(F32, T2, 0, 64), ALU.subtract, r=["T2", "T1"], w=["T1"])
                AE("activation", sb(BF16, T3, [(1, 2048)]), sb(F32, T1, [(1, 2048)]), AF.Exp, r=["T1", "QQ0"], w=["T3"])
                VE("tensor_tensor", sb(BF16, B3, [(1, 2048)]), sb(BF16, BK, [(1, 2048)]), sb(BF16, T3, [(1, 2048)]), ALU.mult, r=["BK", "T3"], w=["B3"])
                for tj in range(16):
                    TE("transpose", ps(2048 + tj * 64, [(1, 128)], BF16), sb(BF16, B3 + tj * 256, [(1, 128)]), ident, r=["B3", "c16"], w=["pT"])
                AE("activation", sb(BF16, KGT, [(1, 2048)]), ps(2048, [(1, 2048)], BF16), AF.Copy, r=["pT"], w=["KGT"])
                VE("tensor_tensor", v3(F32, T2, 32, 32), v3(F32, T2, 32, 32), sb(F32, T2 + 31 * 4, [(64, 32), (0, 32)]), ALU.subtract, r=["T2", "DEC", "T3"], w=["T2"])
                AE("activation", v3(BF16, T3, 32, 32), v3(F32, T2, 32, 32), AF.Exp, r=["T2", "B3"], w=["T3"])
                VE("tensor_tensor", v3(BF16, QQ1, 32, 32), v3(BF16, BQ, 32, 32), v3(BF16, T3, 32, 32), ALU.mult, r=["BQ", "T3"], w=["QQ1"])
                AE("activation", sb(BF16, T3, [(1, 2048)]), sb(F32, T2, [(1, 2048)]), AF.Exp, scale=-1.0, r=["T2", "QQ1"], w=["T3"])
                VE("tensor_tensor", v3(BF16, KK0, 0, 32), v3(BF16, BK, 0, 32), v3(BF16, T3, 0, 32), ALU.mult, r=["BK", "T3"], w=["KK0"])
                VE("tensor_tensor", v3(BF16, KK1, 32, 32), v3(BF16, BK, 32, 32), v3(BF16, T3, 32, 32), ALU.mult, r=["BK", "T3"], w=["KK1"])
                proj_tok(4 * h + 2, 0, "pA")
                AE("activation", sb(BF16, ITK, [(1, 2048)]), ps(0, [(1, 2048)]), AF.Copy, r=["pA"], w=["ITK"])
                for tj in range(16):
                    pa = (6 + tj % 2) * 512
                    po = pa + 128
                    mm(ps(pa, [(1, 128)]), sb(BF16, KK0 + tj * 256, [(1, 128)]), sb(BF16, QQ0 + tj * 256, [(1, 128)]), True, False, ["KK0", "QQ0"], [("pa", tj % 2)])
                    mm(ps(pa, [(1, 128)]), sb(BF16, KK1 + tj * 256, [(1, 128)]), sb(BF16, QQ1 + tj * 256, [(1, 128)]), False, True, ["KK1", "QQ1"], [("pa", tj % 2)])
                    att = sb(BF16, ATT + (tj % 2) * 256, [(1, 128)])
                    VE("tensor_tensor", att, ps(pa, [(1, 128)]), bdm, ALU.mult, r=[("pa", tj % 2), "c16"], w=[("att", tj % 2)])
                    mm(ps(po, [(1, 128)]), sb(BF16, ITK + tj * 256, [(1, 128)]), att, True, False, ["ITK", ("att", tj % 2)], [("po", tj % 2)])
                    for c2 in range(2):
                        n = 2 * tj + c2
                        if n > 0:
                            mm(ps(po + c2 * 64, [(1, 64)]), sb(BF16, S16 + ((n - 1) % 2) * 256, [(1, 128)]), sb(BF16, QQ0 + n * 128, [(1, 64)]), False, c2 == 1,
                               [("S16", (n - 1) % 2), "QQ0"], [("po", tj % 2)])
                        psS = 2048 + (n % 2) * 512
                        mm(ps(psS, [(1, 128)], p0=0), sb(BF16, KGT + tj * 256, [(1, 128)], p0=c2 * 64, npar=64), sb(BF16, ITK + tj * 256, [(1, 128)], p0=c2 * 64, npar=64),
                           True, True, ["KGT", "ITK"], [("pS", n % 2)])
                        if n == 0:
                            VE("tensor_copy", sb(F32, S32, [(1, 128)]), ps(psS, [(1, 128)]), r=[("pS", n % 2)], w=["S32"])
                        else:
                            VE("scalar_tensor_tensor", sb(F32, S32, [(1, 128)]), sb(F32, S32, [(1, 128)]), sb(F32, DEC + 4 * n, [(1, 1)]),
                                                                            ps(psS, [(1, 128)]), ALU.mult, ALU.add, r=[("pS", n % 2), "S32", "DEC"], w=["S32"])
                        if n < 31:
                            AE("activation", sb(BF16, S16 + (n % 2) * 256, [(1, 128)]), sb(F32, S32, [(1, 128)]), AF.Copy, r=["S32"], w=[("S16", n % 2)])
                    AE("activation", sb(F32, T1 + tj * 512, [(1, 128)]), ps(po, [(1, 128)]), AF.Copy, r=[("po", tj % 2), "T3"], w=["T1"])
                proj_feat(4 * h + 3, 0, "pA")
                AE("activation", sb(BF16, B3, [(1, 2048)]), ps(0, [(1, 2048)]), AF.Silu, r=["pA"], w=["B3"])
                for q4 in range(4):
                    AE("activation", sb(F32, T2 + q4 * 2048, [(1, 512)]), sb(F32, T1 + q4 * 2048, [(1, 512)]), AF.Square, r=["T1", "T2"], w=["T2"])
                    mm(ps(q4 * 512, [(1, 512)]), ones32, sb(F32, T2 + q4 * 2048, [(1, 512)]), True, True, ["T2", "c32"], ["pA"])
                AE("activation", sb(F32, T2, [(1, 2048)]), ps(0, [(1, 2048)]), AF.Sqrt, scale=1.0 / 128, bias=EPS, r=["pA"], w=["T2"])
                VE("reciprocal", sb(F32, T2, [(1, 2048)]), sb(F32, T2, [(1, 2048)]), r=["T2"], w=["T2"])
                VE("scalar_tensor_tensor", sb(F32, T1, [(1, 2048)]), sb(F32, T1, [(1, 2048)]), sb(F32, SMALL + 4 * 72, [(1, 1)]), sb(F32, T2, [(1, 2048)]),
                                                  ALU.mult, ALU.mult, r=["T1", "T2", "small"], w=["T1"])
                VE("tensor_tensor", bufT(mbuf, h, 0, 2048), sb(F32, T1, [(1, 2048)]), sb(BF16, B3, [(1, 2048)]), ALU.mult, r=["T1", "B3"], w=[("M", h)])
            P.barrier()
            if stop == "A":
                return finish_early(mbuf, 6)

            CT = RB
            CTK = CT + 8192
            IK2 = CTK + 8192
            SGN = IK2 + 4096
            WAB = SGN + 1024
            WUK = WAB + 1024
            WUV = WUK + 3072
            BI2 = WUV + 3072
            QB = BI2 + 3072
            SC = QB + 6144
            RT = SC + 8192
            MTS = RT + 2048
            QL = MTS + 4096
            LB_ = QL + 3072
            EM = LB_ + 8192
            OLT = EM + 4096
            BST = OLT + 512
            BEND = BST + 512
            assert BEND <= REND, BEND
            IQT = BUF[mbuf] + 14 * 4096
            set_slots([BUF[mbuf] + 12 * 4096, BUF[mbuf] + 13 * 4096])
            dma_pool(sb(BF16, WUK, [(1, 1536)]), dr(wuk_d, l * 128 * 1536, [(1536, 128), (1, 1536)]), w=["WUK"])
            dma_pool(sb(BF16, WUV, [(1, 1536)]), dr(wuv_d, l * 128 * 1536, [(1536, 128), (1, 1536)]), w=["WUV"])
            dma_pool(sb(BF16, BI2, [(1, 1536)]), bias2_d.ap(), w=["BI2"])
            KVG = LB_ + 4096
            dma_sync(sb(F32, KVG, [(1, 256)]), dr(smB_d, l * 128 * 256, [(256, 128), (1, 256)]), w=["KVG"])
            for h in range(6):
                VE("tensor_scalar", sb(BF16, BI2 + h * 512, [(1, 256)]), sb(BF16, BI2 + h * 512, [(1, 256)]), rbc(h), None, ALU.subtract, r=["BI2", "c32"], w=["BI2"])
            SS = BST + 256
            for cc in range(2):
                proj_tok(24 + cc, cc * 2048, ("pck", cc))
            for half in range(2):
                for cc in range(2):
                    AE("activation", sb(F32, SC + cc * 512, [(256, 8), (1, 128)]), ps(cc * 2048 + half * 1024, [(128, 8), (1, 128)]), AF.Copy,
                       r=[("pck", cc)], w=["SC"])
                for t8 in range(8):
                    AE("activation", sb(F32, LB_, [(1, 256)]), sb(F32, SC + t8 * 1024, [(1, 256)]), AF.Square,
                                                              accum_out=sb(F32, SS + 4 * (half * 8 + t8), [(1, 1)]), r=["SC", "LBs"], w=["LBs", "SS"])
                AE("activation", sb(F32, SS + 32 * half, [(1, 8)]), sb(F32, SS + 32 * half, [(1, 8)]), AF.Sqrt, scale=1.0 / 256, bias=EPS, r=["SS"], w=["SS"])
                VE("reciprocal", sb(F32, SS + 32 * half, [(1, 8)]), sb(F32, SS + 32 * half, [(1, 8)]), r=["SS"], w=["SS"])
                for t8 in range(8):
                    tj = half * 8 + t8
                    VE("scalar_tensor_tensor", sb(BF16, CTK + tj * 512, [(1, 256)]), sb(F32, SC + t8 * 1024, [(1, 256)]), sb(F32, SS + 4 * tj, [(1, 1)]),
                                                                     sb(F32, KVG, [(1, 256)]), ALU.mult, ALU.mult, r=["SC", "SS", "KVG"], w=["CTK"])
            for cc in range(2):
                for tj in range(16):
                    TE("transpose", ps(2048 + cc * 1024 + tj * 64, [(1, 128)], BF16), sb(BF16, CTK + tj * 512 + cc * 256, [(1, 128)]), ident,
                       r=["CTK", "c16"], w=["pT4"])
            AE("activation", sb(BF16, CT, [(1, 4096)]), ps(2048, [(1, 4096)], BF16), AF.Copy, r=["pT4"], w=["CT"])
            proj_feat(26, 0, "pA")
            AE("activation", sb(BF16, IK2, [(1, 2048)]), ps(0, [(1, 2048)]), AF.Copy, r=["pA"], w=["IK2"])
            proj_tok(27, 0, "pA", ncol=16)
            VE("tensor_scalar", sb(F32, SGN, [(1, 256)]), ps(0, [(1, 256)]), 0.0, 2.0, ALU.is_gt, ALU.mult, r=["pA"], w=["SGN"])
            VE("tensor_scalar", sb(F32, SGN, [(1, 256)]), sb(F32, SGN, [(1, 256)]), -1.0, None, ALU.add, r=["SGN"], w=["SGN"])
            VE("scalar_tensor_tensor", sb(F32, WAB, [(1, 256)]), ps(0, [(1, 256)]), 1.0 / 32.0, sb(F32, SGN, [(1, 256)]), ALU.mult, ALU.mult, r=["pA", "SGN"], w=["WAB"])

            NIT = 24
            CAND = BST
            CNT = BST + 4
            UU = BST + 8
            RNG = BST + 12
            TAU = BST + 16
            MX = BST + 20
            NMX = BST + 24
            SUM = BST + 28
            RIN = BST + 32
            DTAB = BST + 64
            POW2 = None
            for tt in range(4):
                for b in range(8):
                    off, key = next_w(wblk(win_d, l, 28 + b))
                    pc = (b % 2) * 512
                    for kc in range(16):
                        mm(ps(pc, [(1, 512)]), sb(BF16, off + kc * 256, [(1, 128)]), Hrhs(kc, tt * 512, 512), kc == 0, kc == 15, [key, ("H", kc, tt)], [("pp", b % 2)])
                    AE("activation", sb(BF16, IQT + b * 1024, [(1, 512)]), ps(pc, [(1, 512)]), AF.Copy, r=[("pp", b % 2)], w=["IQT"])
                for b in range(6):
                    off, key = next_w(wblk(win_d, l, 36 + b))
                    pc = (b % 2) * 512
                    for kc in range(16):
                        mm(ps(pc, [(1, 512)]), sb(BF16, off + kc * 256, [(1, 128)]), Hrhs(kc, tt * 512, 512), kc == 0, kc == 15, [key, ("H", kc, tt)], [("pp", b % 2)])
                    AE("activation", sb(BF16, QB + b * 1024, [(1, 512)]), ps(pc, [(1, 512)]), AF.Copy, r=[("pp", b % 2)], w=["QB"])
                for jj in range(4):
                    j = tt * 4 + jj
                    sv = (j + 1) * 128
                    nch = (sv + 511) // 512
                    for hh in range(16):
                        blk, hp = hh // 2, hh % 2
                        for c4 in range(nch):
                            n = min(512, sv - c4 * 512)
                            pi = (hh * nch + c4) % 2
                            pc = pi * 512
                            mm(ps(pc, [(1, n)]), sb(BF16, IQT + blk * 1024 + jj * 256, [(1, 128)], p0=hp * 64, npar=64), sb(BF16, IK2 + c4 * 1024, [(1, n)], p0=hp * 64, npar=64),
                               True, True, ["IQT", "IK2"], [("pp", pi)])
                            rt = sb(BF16, RT + pi * 1024, [(1, n)])
                            AE("activation", rt, ps(pc, [(1, n)]), AF.Relu, scale=sb(F32, WAB + 4 * (j * 16 + hh), [(1, 1)]),
                               r=[("pp", pi), "WAB"], w=[("rt", pi)])
                            scv = sb(F32, SC + c4 * 2048, [(1, n)])
                            if hh == 0:
                                VE("tensor_scalar", scv, rt, sb(F32, SGN + 4 * (j * 16 + hh), [(1, 1)]), None, ALU.mult,
                                   r=[("rt", pi), "SGN"], w=["SC"])
                            else:
                                VE("scalar_tensor_tensor", scv, rt, sb(F32, SGN + 4 * (j * 16 + hh), [(1, 1)]), scv, ALU.mult, ALU.add,
                                   r=[("rt", pi), "SGN", "SC"], w=["SC"])
                    VE("tensor_tensor", sb(F32, SC + j * 512, [(1, 128)]), sb(F32, SC + j * 512, [(1, 128)]), negm, ALU.add, r=["SC", "c32"], w=["SC"])
                    scf = sb(F32, SC, [(1, sv)])
                    if j >= 2:
                        VE("tensor_reduce", sb(F32, MX, [(1, 1)]), scf, AX.X, ALU.max, r=["SC"], w=["bs"])
                        VE("tensor_scalar", sb(F32, LB_, [(1, sv)]), scf, -1.0e4, None, ALU.max, r=["SC", "LBs"], w=["LBs"])
                        VE("tensor_reduce", sb(F32, TAU, [(1, 1)]), sb(F32, LB_, [(1, sv)]), AX.X, ALU.min, r=["LBs", "bs"], w=["bs"])
                        VE("tensor_tensor", sb(F32, RNG, [(1, 1)]), sb(F32, MX, [(1, 1)]), sb(F32, TAU, [(1, 1)]), ALU.subtract, r=["bs"], w=["bs"])
                        VE("tensor_scalar", sb(F32, RNG, [(1, 1)]), sb(F32, RNG, [(1, 1)]), 0.5, 1e-6, ALU.mult, ALU.add, r=["bs"], w=["bs"])
                        VE("tensor_tensor", sb(F32, CAND, [(1, 1)]), sb(F32, TAU, [(1, 1)]), sb(F32, RNG, [(1, 1)]), ALU.add, r=["bs"], w=["bs"])
                        for i in range(NIT + 1):
                            VE("tensor_scalar", sb(F32, DTAB + 4 * i, [(1, 1)]), sb(F32, RNG, [(1, 1)]), float(2.0 ** -i), None, ALU.mult, r=["bs"], w=["dtab"])
                        for i in range(NIT):
                            VE("tensor_scalar", sb(BF16, EM, [(1, sv)]), scf, sb(F32, CAND, [(1, 1)]), 0.0, ALU.is_ge, ALU.add,
                                                                        accum_out=sb(F32, CNT, [(1, 1)]), r=["SC", "bs", "EM"], w=["EM", "cnt"])
                            VE("scalar_tensor_tensor", sb(F32, UU, [(1, 1)]), sb(F32, CNT, [(1, 1)]), 255.5, sb(F32, DTAB + 4 * i, [(1, 1)]), ALU.is_ge, ALU.mult,
                               r=["cnt", "dtab"], w=["uu"])
                            VE("scalar_tensor_tensor", sb(F32, CAND, [(1, 1)]), sb(F32, UU, [(1, 1)]), sb(F32, CAND, [(1, 1)]), sb(F32, DTAB + 4 * (i + 1), [(1, 1)]),
                                                                    ALU.add, ALU.subtract, r=["uu", "bs", "dtab"], w=["bs"])
                        VE("tensor_tensor", sb(F32, TAU, [(1, 1)]), sb(F32, CAND, [(1, 1)]), sb(F32, DTAB + 4 * NIT, [(1, 1)]), ALU.subtract, r=["bs", "dtab"], w=["tau"])
                        VE("tensor_scalar", sb(BF16, MTS, [(1, sv)]), scf, sb(F32, TAU, [(1, 1)]), None, ALU.is_ge, r=["SC", "tau"], w=["MTS"])
                    else:
                        VE("tensor_scalar", sb(BF16, MTS, [(1, sv)]), scf, -1.0e29, None, ALU.is_ge, r=["SC"], w=["MTS"])
                    for cc in range(2):
                        for h in range(6):
                            mm(ps(1024 + h * 128, [(1, 128)]), sb(BF16, WUK + h * 512 + cc * 256, [(1, 128)]), sb(BF16, QB + h * 1024 + jj * 256, [(1, 128)]), True, True,
                               ["WUK", "QB"], ["pql", "pl1"])
                        AE("activation", sb(BF16, QL + cc * 1536, [(1, 768)]), ps(1024, [(1, 768)]), AF.Copy, scale=float(128 ** -0.5), r=["pql", "pl1"], w=["QL"])
                    for h in range(6):
                        for c4 in range(nch):
                            n = min(512, sv - c4 * 512)
                            pi = c4 % 2
                            pc = 1024 + pi * 512
                            for cc in range(2):
                                mm(ps(pc, [(1, n)]), sb(BF16, QL + cc * 1536 + h * 256, [(1, 128)]), sb(BF16, CT + cc * 4096 + c4 * 1024, [(1, n)]), cc == 0, cc == 1,
                                   ["QL", "CT"], ["pql" if pi == 0 else "pl1"])
                            AE("activation", sb(F32, LB_ + c4 * 2048, [(1, n)]), ps(pc, [(1, n)]), AF.Identity, bias=rbc(h),
                               r=["pql" if pi == 0 else "pl1", "c32", "LBs"], w=["LBs"])
                        if j == 0:
                            VE("tensor_tensor", sb(F32, LB_, [(1, 128)]), sb(F32, LB_, [(1, 128)]), sb(BF16, BI2 + h * 512 + 256, [(1, 128)]), ALU.add, r=["LBs", "BI2"], w=["LBs"])
                        else:
                            VE("tensor_tensor", sb(F32, LB_ + (j - 1) * 512, [(1, 256)]), sb(F32, LB_ + (j - 1) * 512, [(1, 256)]), sb(BF16, BI2 + h * 512, [(1, 256)]), ALU.add,
                               r=["LBs", "BI2"], w=["LBs"])
                        lf = sb(F32, LB_, [(1, sv)])
                        VE("tensor_reduce", sb(F32, MX, [(1, 1)]), lf, AX.X, ALU.max, r=["LBs"], w=["mx"])
                        VE("tensor_scalar", sb(F32, NMX, [(1, 1)]), sb(F32, MX, [(1, 1)]), -1.0, None, ALU.mult, r=["mx"], w=["nmx"])
                        AE("activation", lf, lf, AF.Exp, bias=sb(F32, NMX, [(1, 1)]), r=["LBs", "nmx"], w=["LBs"])
                        VE("scalar_tensor_tensor", sb(BF16, EM, [(1, sv)]), lf, 1.0, sb(BF16, MTS, [(1, sv)]), ALU.mult, ALU.mult, accum_out=sb(F32, SUM, [(1, 1)]),
                           r=["LBs", "MTS", "EM"], w=["EM", "sum"])
                        VE("reciprocal", sb(F32, RIN, [(1, 1)]), sb(F32, SUM, [(1, 1)]), r=["sum"], w=["rin"])
                        VE("tensor_scalar", sb(BF16, EM, [(1, sv)]), sb(BF16, EM, [(1, sv)]), sb(F32, RIN, [(1, 1)]), None, ALU.mult, r=["EM", "rin"], w=["EM"])
                        for i in range(j + 1):
                            TE("transpose", ps(2048 + i * 64, [(1, 128)], BF16), sb(BF16, EM + i * 256, [(1, 128)]), ident, r=["EM", "c16"], w=["pT"])
                        AE("activation", sb(BF16, EM, [(1, sv)]), ps(2048, [(1, sv)], BF16), AF.Copy, r=["pT", "EM"], w=["EM"])
                        for cc in range(2):
                            for i in range(j + 1):
                                mm(ps(3072 + cc * 128, [(1, 128)]), sb(BF16, CTK + i * 512 + cc * 256, [(1, 128)]), sb(BF16, EM + i * 256, [(1, 128)]), i == 0, i == j,
                                   ["CTK", "EM"], ["pol"])
                        AE("activation", sb(BF16, OLT, [(1, 256)]), ps(3072, [(1, 256)]), AF.Copy, r=["pol"], w=["OLT"])
                        for cc in range(2):
                            mm(ps(3584, [(1, 128)]), sb(BF16, WUV + h * 512 + cc * 256, [(1, 128)]), sb(BF16, OLT + cc * 256, [(1, 128)]), cc == 0, cc == 1, ["WUV", "OLT"], ["pov"])
                        AE("activation", bufT(mbuf, 6 + h, j * 128, 128), ps(3584, [(1, 128)]), AF.Copy, r=["pov"], w=[("M", 6 + h)])
            P.barrier()
            if stop == "B":
                return finish_early(mbuf, 12)

            UT = RB
            VV = UT + 4096
            VQ = VV + 8192
            VN = VQ + 8192
            WST = VN + 4096
            GBR = WST + 1024
            GBH = GBR + 2048
            GBL = GBH + 1024
            ST1 = GBL + 1024
            CW0 = ST1 + 256
            set_slots([CW0 + i * 4096 for i in range(8)])
            assert CW0 + 8 * 4096 <= REND
            dma_pool(sb(BF16, WST, [(1, 512)]), dr(wsT_d, l * 128 * 512, [(512, 128), (1, 512)]), w=["WST"])
            dma_sync(sb(F32, GBR, [(1, 512)], npar=1), dr(gb_d, l * 512, [(512, 1), (1, 512)]), w=["GBR"])
            for g in range(4):
                VE("tensor_tensor", sb(BF16, WST + g * 256, [(1, 128)]), sb(BF16, WST + g * 256, [(1, 128)]), triu, ALU.mult, r=["WST", "c16"], w=["WST"])
            VE("tensor_copy", sb(BF16, GBH, [(1, 512)], npar=1), sb(F32, GBR, [(1, 512)], npar=1), r=["GBR"], w=["GBH"])
            VE("tensor_tensor", sb(BF16, GBL, [(1, 512)], npar=1), sb(F32, GBR, [(1, 512)], npar=1), sb(BF16, GBH, [(1, 512)], npar=1), ALU.subtract, r=["GBR", "GBH"], w=["GBL"])
            for g in range(4):
                proj_feat(42 + 2 * g, 0, "pA")
                AE("activation", sb(BF16, UT, [(1, 2048)]), ps(0, [(1, 2048)]), AF.Gelu_apprx_tanh, r=["pA"], w=["UT"])
                proj_tok(42 + 2 * g + 1, 2048, "pB")
                AE("activation", sb(F32, VV, [(1, 2048)]), ps(2048, [(1, 2048)]), AF.Gelu_apprx_tanh, r=["pB"], w=["VV"])
                AE("activation", sb(F32, VQ, [(1, 2048)]), sb(F32, VV, [(1, 2048)]), AF.Square, r=["VV"], w=["VQ"])
                VE("tensor_reduce", sb(F32, ST1, [(1, 16)]), sb(F32, VV, [(128, 16), (1, 128)]), AX.X, ALU.add, r=["VV"], w=["s1"])
                VE("tensor_reduce", sb(F32, ST1 + 64, [(1, 16)]), sb(F32, VQ, [(128, 16), (1, 128)]), AX.X, ALU.add, r=["VQ"], w=["s2"])
                VE("tensor_scalar", sb(F32, ST1 + 128, [(1, 16)]), sb(F32, ST1, [(1, 16)]), 1.0 / 128, None, ALU.mult, r=["s1"], w=["mean"])
                VE("tensor_tensor", sb(F32, ST1, [(1, 16)]), sb(F32, ST1 + 128, [(1, 16)]), sb(F32, ST1 + 128, [(1, 16)]), ALU.mult, r=["mean", "s1"], w=["s1"])
                VE("scalar_tensor_tensor", sb(F32, ST1 + 192, [(1, 16)]), sb(F32, ST1 + 64, [(1, 16)]), 1.0 / 128, sb(F32, ST1, [(1, 16)]), ALU.mult, ALU.subtract,
                   r=["s1", "s2"], w=["var"])
                AE("activation", sb(F32, ST1 + 192, [(1, 16)]), sb(F32, ST1 + 192, [(1, 16)]), AF.Sqrt, bias=EPS, r=["var"], w=["var"])
                VE("reciprocal", sb(F32, ST1 + 192, [(1, 16)]), sb(F32, ST1 + 192, [(1, 16)]), r=["var"], w=["var"])
                for n in range(16):
                    VE("tensor_scalar", sb(BF16, VN + n * 256, [(1, 128)]), sb(F32, VV + n * 512, [(1, 128)]), sb(F32, ST1 + 128 + 4 * n, [(1, 1)]),
                                                     sb(F32, ST1 + 192 + 4 * n, [(1, 1)]), ALU.subtract, ALU.mult, r=["VV", "mean", "var"], w=["VN"])
                for n in range(16):
                    mm(ps(n * 128, [(1, 128)]), sb(BF16, VN + n * 256, [(1, 128)]), sb(BF16, WST + g * 256, [(1, 128)]), True, False, ["VN", "WST"], ["pA"])
                    mm(ps(n * 128, [(1, 128)]), sb(BF16, TRIU, [(1, 128)], npar=1), sb(BF16, GBH + g * 256, [(1, 128)], npar=1), False, False, ["GBH", "c16"], ["pA"])
                    mm(ps(n * 128, [(1, 128)]), sb(BF16, TRIU, [(1, 128)], npar=1), sb(BF16, GBL + g * 256, [(1, 128)], npar=1), False, True, ["GBL", "c16"], ["pA"])
                VE("tensor_tensor", bufT(mbuf, 12 + g, 0, 2048), ps(0, [(1, 2048)]), sb(BF16, UT, [(1, 2048)]), ALU.mult, r=["pA", "UT"], w=[("M", 12 + g)])
            P.barrier()
            if stop == "C":
                return finish_early(mbuf)
            if debug and l == 0:
                dma_pool(dbg_d.ap(), sb(BF16, BUF[mbuf], [(1, 32768)]), r=[("M", i) for i in range(16)], w=["dbg"])
                P.barrier()

            ACT = BUF[hb]
            H2 = ACT + 45056
            XT = RB
            AB = XT + 32768
            TG = AB + 4 * 2064
            SQT = TG + 4 * 2048
            RST = SQT + 2048
            CVW = RST + 2048
            HAL = CVW + 1408
            TW0 = HAL + 704
            TW0 = (TW0 + 31) // 32 * 32
            nsl = (REND - TW0) // 4096
            assert nsl >= 4, nsl
            set_slots([TW0 + i * 4096 for i in range(min(nsl, 8))])
            dma_sync(sb(F32, CVW, [(1, 352)]), dr(smT_d, l * 128 * 352, [(352, 128), (1, 352)]), w=["CVW"])
            VE("memset", sb(F32, HAL, [(1, 176)]), 0.0, w=["HAL"])
            last_layer = (l == L - 1)
            for tt in range(4):
                dma_sync(sb(F32, XT, [(512, 16), (1, 512)]), dr(xsrc, (bi * D * S if l == 0 else 0) + tt * 512, [(S, 128), (128 * S, 16), (1, 512)]), r=["xscr"] if l > 0 else [], w=[("X", ob) for ob in range(16)])
                for ob in range(16):
                    off, key = next_w(wblk(wout_d, l, ob))
                    pc = (ob % 2) * 512
                    for kc in range(16):
                        mm(ps(pc, [(1, 512)]), sb(BF16, off + kc * 256, [(1, 128)]), bufT(mbuf, kc, tt * 512, 512), kc == 0, kc == 15, [key, ("M", kc)], [("pd", ob % 2)])
                    xo = sb(F32, XT + ob * 2048, [(1, 512)])
                    VE("tensor_tensor", xo, ps(pc, [(1, 512)]), xo, ALU.add, r=[("pd", ob % 2), ("X", ob)], w=[("X", ob)])
                    AE("activation", sb(F32, SQT, [(1, 512)]), xo, AF.Square, r=[("X", ob)], w=["SQT"])
                    mm(ps(1024, [(1, 512)]), ones32, sb(F32, SQT, [(1, 512)]), ob == 0, ob == 15, ["SQT", "c32"], ["pss"])
                AE("activation", sb(F32, RST, [(1, 512)]), ps(1024, [(1, 512)]), AF.Sqrt, scale=1.0 / D, bias=EPS, r=["pss"], w=["RST"])
                VE("reciprocal", sb(F32, RST, [(1, 512)]), sb(F32, RST, [(1, 512)]), r=["RST"], w=["RST"])
                for kc in range(16):
                    VE("scalar_tensor_tensor", sb(BF16, H2 + kc * 1024, [(1, 512)]), sb(F32, XT + kc * 2048, [(1, 512)]), gcol(1, kc), sb(F32, RST, [(1, 512)]),
                                                              ALU.mult, ALU.mult, r=[("X", kc), "RST", "small"], w=[("H2", kc)])
                for fc in range(NFC):
                    d2 = fc % 2
                    tbuf = {}
                    for gu in range(2):
                        blk = fc + gu * NFC
                        off, key = next_w(wblk(wup_d, l, blk))
                        pc = 1536 + (2 * d2 + gu) * 512
                        pk = ("pu", 2 * d2 + gu)
                        for kc in range(16):
                            mm(ps(pc, [(1, 512)]), sb(BF16, off + kc * 256, [(1, 128)]), sb(BF16, H2 + kc * 1024, [(1, 512)]), kc == 0, kc == 15, [key, ("H2", kc)], [pk])
                        ab = AB + (2 * d2 + gu) * 2064
                        abk = ("ab", 2 * d2 + gu)
                        tg = TG + (2 * d2 + gu) * 2048
                        tk = ("tg", 2 * d2 + gu)
                        VE("tensor_copy", sb(F32, ab, [(1, 2)]), sb(F32, HAL + blk * 8, [(1, 2)]), r=["HAL"], w=[abk])
                        AE("activation", sb(F32, ab + 8, [(1, 512)]), ps(pc, [(1, 512)]), AF.Copy, r=[pk, abk], w=[abk])
                        VE("tensor_copy", sb(F32, HAL + blk * 8, [(1, 2)]), sb(F32, ab + 2048, [(1, 2)]), r=[abk], w=["HAL"])
                        AE("activation", sb(F32, tg, [(1, 512)]), sb(F32, ab + 8, [(1, 512)]), AF.Identity,
                                                                        scale=sb(F32, CVW + 4 * (2 * 88 + blk), [(1, 1)]), bias=sb(F32, CVW + 4 * (264 + blk), [(1, 1)]),
                           r=[abk, "CVW"], w=[tk])
                        VE("scalar_tensor_tensor", sb(F32, tg, [(1, 512)]), sb(F32, ab + 4, [(1, 512)]), sb(F32, CVW + 4 * (88 + blk), [(1, 1)]),
                                                                                  sb(F32, tg, [(1, 512)]), ALU.mult, ALU.add, r=[abk, tk, "CVW"], w=[tk])
                        VE("scalar_tensor_tensor", sb(F32, tg, [(1, 512)]), sb(F32, ab, [(1, 512)]), sb(F32, CVW + 4 * blk, [(1, 1)]),
                                                                                  sb(F32, tg, [(1, 512)]), ALU.mult, ALU.add, r=[abk, tk, "CVW"], w=[tk])
                        tbuf[gu] = (tg, tk)
                    (tgg, tkg), (tgu, tku) = tbuf[0], tbuf[1]
                    AE("activation", sb(F32, tgg, [(1, 512)]), sb(F32, tgg, [(1, 512)]), AF.Silu, r=[tkg], w=[tkg])
                    VE("tensor_tensor", sb(BF16, ACT + fc * 1024, [(1, 512)]), sb(F32, tgg, [(1, 512)]), sb(F32, tgu, [(1, 512)]), ALU.mult,
                       r=[tkg, tku], w=[("ACT", fc)])
                for ob in range(16):
                    pc = (ob % 2) * 512
                    for sbk in range(4):
                        off, key = next_w(dr(wdn_d, ((l * 64) + ob * 4 + sbk) * 128 * 1408, [(1408, 128), (1, 1408)]), ncols=1408)
                        for k11 in range(11):
                            fcx = sbk * 11 + k11
                            mm(ps(pc, [(1, 512)]), sb(BF16, off + k11 * 256, [(1, 128)]), sb(BF16, ACT + fcx * 1024, [(1, 512)]), fcx == 0, fcx == 43, [key, ("ACT", fcx)], [("pd", ob % 2)])
                    xo = sb(F32, XT + ob * 2048, [(1, 512)])
                    VE("tensor_tensor", xo, ps(pc, [(1, 512)]), xo, ALU.add, r=[("pd", ob % 2), ("X", ob)], w=[("X", ob)])
                    AE("activation", sb(F32, SQT, [(1, 512)]), xo, AF.Square, r=[("X", ob)], w=["SQT"])
                    mm(ps(1024, [(1, 512)]), ones32, sb(F32, SQT, [(1, 512)]), ob == 0, ob == 15, ["SQT", "c32"], ["pss"])
                if not (last_layer and final):
                    dma_sync(dr(xs_d, tt * 512, [(S, 128), (128 * S, 16), (1, 512)]), sb(F32, XT, [(512, 16), (1, 512)]), r=[("X", ob) for ob in range(16)], w=["xscr"])
                AE("activation", sb(F32, RST, [(1, 512)]), ps(1024, [(1, 512)]), AF.Sqrt, scale=1.0 / D, bias=EPS, r=["pss"], w=["RST"])
                VE("reciprocal", sb(F32, RST, [(1, 512)]), sb(F32, RST, [(1, 512)]), r=["RST"], w=["RST"])
                if last_layer:
                    if final:
                        for kc in range(16):
                            VE("scalar_tensor_tensor", sb(F32, XT + kc * 2048, [(1, 512)]), sb(F32, XT + kc * 2048, [(1, 512)]), gcol(2, kc), sb(F32, RST, [(1, 512)]),
                                                                      ALU.mult, ALU.mult, r=[("X", kc), "RST", "small"], w=[("X", kc)])
                        rk = [("X", kc) for kc in range(16)]
                    else:
                        rk = [("X", kc) for kc in range(16)]
                    lastdma = dma_sync(dr(out_d, bi * D * S + tt * 512, [(S, 128), (128 * S, 16), (1, 512)]), sb(F32, XT, [(512, 16), (1, 512)]), r=rk, w=["outd"])
                else:
                    for kc in range(16):
                        VE("scalar_tensor_tensor", bufT(mbuf, kc, tt * 512, 512), sb(F32, XT + kc * 2048, [(1, 512)]), gcol(2, kc), sb(F32, RST, [(1, 512)]),
                                                                         ALU.mult, ALU.mult, r=[("X", kc), "RST", "small"], w=[("M", kc)])
            P.barrier()
            xsrc = xs_d

    P.emit({})
    return nc


def ones16_row():
    raise RuntimeError("unused")


def _blk(W, cols):
    sub = W[:, cols]
    if sub.shape[1] < 128:
        sub = np.concatenate([sub, np.zeros((sub.shape[0], 128 - sub.shape[1]), W.dtype)], axis=1)
    return sub.reshape(16, 128, 128).transpose(1, 0, 2).reshape(128, 2048)


def _t5_bucket(dist):
    import math
    d = np.maximum(dist, 0)
    d_f = np.maximum(d, 1).astype(np.float32)
    large = 16 + (np.log(d_f / 16) / math.log(128 / 16) * 16).astype(np.int32)
    large = np.minimum(large, 31)
    return np.where(d < 16, d, large)


def _prep(inputs, layers):
    L = len(layers)
    w_in = inputs["w_in"]
    r = np.arange
    win = np.empty((L, NB_IN, 128, 2048), np.float32)
    for li, l in enumerate(layers):
        W = w_in[l]
        b = 0
        for h in range(6):
            for base in (A_Q, A_F, A_I, A_G):
                win[li, b] = _blk(W, base + h * 128 + r(128)); b += 1
        win[li, b] = _blk(W, B_CKV + r(128)); b += 1
        win[li, b] = _blk(W, B_CKV + 128 + r(128)); b += 1
        win[li, b] = _blk(W, np.concatenate([I_K + r(64), I_K + r(64)])); b += 1
        win[li, b] = _blk(W, I_W + r(16)); b += 1
        for k in range(8):
            win[li, b] = _blk(W, I_Q + k * 128 + r(128)); b += 1
        for k in range(6):
            win[li, b] = _blk(W, B_Q + k * 128 + r(128)); b += 1
        for g in range(4):
            win[li, b] = _blk(W, C_U + g * 128 + r(128)); b += 1
            win[li, b] = _blk(W, C_V + g * 128 + r(128)); b += 1
        assert b == NB_IN
    wout = np.stack([inputs["w_out"][l].reshape(16, 128, 16, 128).transpose(2, 1, 0, 3).reshape(16, 128, 2048) for l in layers])
    wup = np.stack([inputs["w_up"][l].reshape(16, 128, 88, 128).transpose(2, 1, 0, 3).reshape(88, 128, 2048) for l in layers])
    wdn = np.stack([inputs["w_down"][l].reshape(4, 11, 128, 16, 128).transpose(3, 0, 2, 1, 4).reshape(64, 128, 1408) for l in layers])
    NSM = 73
    sm = np.zeros((L, 128, NSM), np.float32)
    smB = np.zeros((L, 128, 256), np.float32)
    smT = np.zeros((L, 128, 352), np.float32)
    gb = np.zeros((L, 1, 512), np.float32)
    wuk = np.zeros((L, 128, 1536), np.float32)
    wuv = np.zeros((L, 128, 1536), np.float32)
    wsT = np.zeros((L, 128, 512), np.float32)
    for li, l in enumerate(layers):
        sm[li, :, 0:16] = inputs["norm_mix"][l].reshape(16, 128).T
        sm[li, :, 16:32] = inputs["norm_ffn"][l].reshape(16, 128).T
        nxt = inputs["norm_mix"][l + 1] if l + 1 < DEPTH else inputs["norm_final"]
        sm[li, :, 32:48] = nxt.reshape(16, 128).T
        sm[li, :, 48:72] = inputs["lower_bounds"].reshape(4, 6, 128).transpose(2, 0, 1).reshape(128, 24)
        sm[li, :, 72] = inputs["a_out_gain"][l]
        smB[li] = np.broadcast_to(inputs["kv_gain"][l][None, :], (128, 256))
        smT[li, :, 0:264] = inputs["conv_w"][l].reshape(3, 88, 128).transpose(2, 0, 1).reshape(128, 264)
        smT[li, :, 264:352] = inputs["conv_b"][l].reshape(88, 128).T
        gb[li, 0] = inputs["gmlp_b"][l].reshape(512)
        wuk[li] = inputs["w_uk"][l].transpose(2, 0, 1).reshape(128, 1536)
        wuv[li] = inputs["w_uv"][l].reshape(6, 2, 128, 128).transpose(2, 0, 1, 3).reshape(128, 1536)
        wsT[li] = inputs["gmlp_w"][l].transpose(2, 0, 1).reshape(128, 512)
    tl = np.arange(128)[:, None]
    s2 = np.arange(256)[None, :]
    bucket = _t5_bucket(128 + tl - s2)
    bias2 = inputs["rel_bias"][bucket]
    bias2 = np.ascontiguousarray(bias2.transpose(0, 2, 1)).reshape(128, 1536).astype(np.float32)
    c32 = np.zeros((128, 262), np.float32)
    c32[:, 0:128] = 1.0
    c32[:, 128:256] = np.where(np.arange(128)[None, :] <= np.arange(128)[:, None], 0.0, -1.0e30)
    c32[:, 256:262] = np.broadcast_to(inputs["rel_bias"][31][None, :], (128, 6))
    c16 = np.zeros((128, 2432), np.float32)
    c16[:, 0:128] = np.eye(128)
    sidx = np.arange(128)[:, None]
    tidx = np.arange(128)[None, :]
    c16[:, 128:256] = ((sidx <= tidx) & (sidx // 64 == tidx // 64)).astype(np.float32)
    c16[:, 256:384] = (sidx <= tidx).astype(np.float32)
    c16[:, 384:2432] = (np.arange(2048) % 64 != 0).astype(np.float32)[None, :]
    return dict(win=win, wout=wout, wup=wup, wdn=wdn, smalls=sm, smallsB=smB, smallsT=smT, gbrow=gb, wukT=wuk, wuv=wuv, wsT=wsT,
                bias2=bias2, c32=c32, c16=c16)


_NC_CACHE = {}


def kernel(**inputs):
    inputs = {k: np.asarray(v) for k, v in inputs.items()}
    x = inputs["x"].astype(np.float32)
    shared = _prep(inputs, list(range(DEPTH)))
    NBC = 2
    ncore = 4 // NBC
    if "full" not in _NC_CACHE:
        _NC_CACHE["full"] = build(DEPTH, 0, True, nb=NBC)
    nc = _NC_CACHE["full"]
    in_maps = []
    for c in range(ncore):
        m = dict(shared)
        m["xT"] = np.ascontiguousarray(np.concatenate([x[c * NBC + b].T for b in range(NBC)], axis=0))
        in_maps.append(m)
    res = run_bass_kernel_spmd(nc, in_maps, core_ids=list(range(ncore)))
    o = np.concatenate([res.results[c]["outT"].reshape(NBC, D, S) for c in range(ncore)], axis=0)
    out = np.ascontiguousarray(o.transpose(0, 2, 1)).astype(np.float32)
    return out
```
